# Optimizing a Trainium2 kernel written in Bass

```python
import math
import jax, jax.numpy as jnp
from jax import lax
import numpy as np

D_MODEL = 4096
BATCH = 2
SEQ = 8192
DEPTH = 2

DN_HEAD_DIM = 128
DN_K_HEADS = D_MODEL // 256
DN_V_HEADS = D_MODEL // 128
DN_CONV = 4
DN_CHUNK = 64
DN_KEY_DIM = DN_K_HEADS * DN_HEAD_DIM
DN_VAL_DIM = DN_V_HEADS * DN_HEAD_DIM
DN_IN_DIM = 2 * DN_KEY_DIM + 2 * DN_VAL_DIM + 2 * DN_V_HEADS
MB_HEAD_DIM = 128
MB_HEADS = D_MODEL // 128
MB_BLOCK = 256
MB_TOPK = 3
MB_Q_CHUNK = 32
MB_WIDTH = MB_HEADS * MB_HEAD_DIM
N_EXPERTS = 32
TOP_K = 4
D_EXPERT = 768
SWIGLU_LIMIT = 7.0
SWIGLU_ALPHA = 1.702
MOE_BLOCK = 128
LN_EPS = 1e-5
NORM_EPS = 1e-6
DEEPNORM_ALPHA = (2 * DEPTH) ** 0.25
DEEPNORM_BETA = (8 * DEPTH) ** -0.25
NEG_INF = -1e30

kernel_name = 'hybrid_deltanet_moba_moe_deepnorm'


def layer_norm(x, g, b):
    xf = x.astype(jnp.float32)
    mu = jnp.mean(xf, axis=-1, keepdims=True)
    xc = xf - mu
    var = jnp.mean(xc * xc, axis=-1, keepdims=True)
    return (xc * lax.rsqrt(var + LN_EPS) * g.astype(jnp.float32) + b.astype(jnp.float32)).astype(x.dtype)


def l2_normalize(x):
    return x * lax.rsqrt(jnp.sum(x * x, axis=-1, keepdims=True) + NORM_EPS)


def causal_depthwise_conv(x, w):
    k_w, c = w.shape
    return lax.conv_general_dilated(x, w[:, None, :].astype(x.dtype), window_strides=(1,),
                                    padding=((k_w - 1, 0),), dimension_numbers=('NWC', 'WIO', 'NWC'),
                                    feature_group_count=c)


def chunked_gated_delta_rule(q, k, v, g, beta):
    bsz, h, t_len, dk = q.shape
    dv = v.shape[-1]
    c = DN_CHUNK
    n = t_len // c
    q = q.reshape(bsz, h, n, c, dk)
    k = k.reshape(bsz, h, n, c, dk)
    v = v.reshape(bsz, h, n, c, dv)
    g = jnp.cumsum(g.reshape(bsz, h, n, c), axis=-1)
    beta = beta.reshape(bsz, h, n, c)
    causal = jnp.tril(jnp.ones((c, c), dtype=bool))
    strict = jnp.tril(jnp.ones((c, c), dtype=bool), -1)
    diff = g[..., :, None] - g[..., None, :]
    decay = jnp.where(causal, jnp.exp(jnp.where(causal, diff, 0.0)), 0.0)
    k_beta = k * beta[..., None]
    v_beta = v * beta[..., None]
    lower = jnp.where(strict, jnp.einsum('bhnid,bhnjd->bhnij', k_beta, k) * decay, 0.0)
    a_mat = lower + jnp.eye(c, dtype=jnp.float32)
    u = lax.linalg.triangular_solve(a_mat, v_beta, left_side=True, lower=True, unit_diagonal=True)
    w = lax.linalg.triangular_solve(a_mat, k_beta * jnp.exp(g)[..., None], left_side=True, lower=True,
                                    unit_diagonal=True)
    intra = jnp.where(causal, jnp.einsum('bhnid,bhnjd->bhnij', q, k) * decay, 0.0)
    q_dec = q * jnp.exp(g)[..., None]
    k_dec = k * jnp.exp(g[..., -1:] - g)[..., None]
    g_tail = jnp.exp(g[..., -1])

    def step(state, inp):
        u_c, w_c, qd_c, kd_c, a_c, gt_c = inp
        v_new = u_c - jnp.einsum('bhck,bhkv->bhcv', w_c, state)
        o_c = jnp.einsum('bhck,bhkv->bhcv', qd_c, state) + jnp.einsum('bhij,bhjv->bhiv', a_c, v_new)
        state = state * gt_c[..., None, None] + jnp.einsum('bhck,bhcv->bhkv', kd_c, v_new)
        return state, o_c

    xs = (jnp.moveaxis(u, 2, 0), jnp.moveaxis(w, 2, 0), jnp.moveaxis(q_dec, 2, 0),
          jnp.moveaxis(k_dec, 2, 0), jnp.moveaxis(intra, 2, 0), jnp.moveaxis(g_tail, 2, 0))
    s0 = jnp.zeros((bsz, h, dk, dv), jnp.float32)
    _, o = lax.scan(step, s0, xs)
    return jnp.moveaxis(o, 0, 2).reshape(bsz, h, t_len, dv)


def gated_deltanet_mixer(x, w_in, conv_w, a_log, dt_bias, norm_w, w_out):
    bsz, t_len, _ = x.shape
    kd, vd = DN_KEY_DIM, DN_VAL_DIM
    proj = x @ w_in
    qkv = jax.nn.silu(causal_depthwise_conv(proj[..., :2 * kd + vd], conv_w))
    z = proj[..., 2 * kd + vd:2 * kd + 2 * vd]
    b_raw = proj[..., 2 * kd + 2 * vd:2 * kd + 2 * vd + DN_V_HEADS]
    a_raw = proj[..., 2 * kd + 2 * vd + DN_V_HEADS:]
    rep = DN_V_HEADS // DN_K_HEADS
    q = qkv[..., :kd].reshape(bsz, t_len, DN_K_HEADS, DN_HEAD_DIM).astype(jnp.float32)
    k = qkv[..., kd:2 * kd].reshape(bsz, t_len, DN_K_HEADS, DN_HEAD_DIM).astype(jnp.float32)
    v = qkv[..., 2 * kd:].reshape(bsz, t_len, DN_V_HEADS, DN_HEAD_DIM).astype(jnp.float32)
    q = jnp.repeat(l2_normalize(q), rep, axis=2) * (DN_HEAD_DIM ** -0.5)
    k = jnp.repeat(l2_normalize(k), rep, axis=2)
    beta = jax.nn.sigmoid(b_raw.astype(jnp.float32))
    g = -jnp.exp(a_log.astype(jnp.float32)) * jax.nn.softplus(a_raw.astype(jnp.float32) + dt_bias.astype(jnp.float32))
    o = chunked_gated_delta_rule(jnp.transpose(q, (0, 2, 1, 3)), jnp.transpose(k, (0, 2, 1, 3)),
                                 jnp.transpose(v, (0, 2, 1, 3)), jnp.transpose(g, (0, 2, 1)),
                                 jnp.transpose(beta, (0, 2, 1)))
    o = jnp.transpose(o, (0, 2, 1, 3))
    zf = z.reshape(bsz, t_len, DN_V_HEADS, DN_HEAD_DIM).astype(jnp.float32)
    o = o * lax.rsqrt(jnp.mean(o * o, axis=-1, keepdims=True) + NORM_EPS) * norm_w.astype(jnp.float32) * jax.nn.silu(zf)
    return o.reshape(bsz, t_len, vd).astype(x.dtype) @ w_out


def alibi_slopes(n_heads):
    return jnp.exp2(-8.0 * jnp.arange(1, n_heads + 1, dtype=jnp.float32) / n_heads)


def moba_attention(q, k, v):
    bsz, h, t_len, dh = q.shape
    nb = t_len // MB_BLOCK
    n_sel = min(MB_TOPK, nb)
    nq = t_len // MB_Q_CHUNK
    scale = dh ** -0.5
    kb = k.reshape(bsz, h, nb, MB_BLOCK, dh)
    vb = v.reshape(bsz, h, nb, MB_BLOCK, dh)
    k_mean = jnp.mean(kb.astype(jnp.float32), axis=3)
    slopes = alibi_slopes(h)[None, :, None]
    bi = jnp.arange(bsz)[:, None, None, None]
    hi = jnp.arange(h)[None, :, None, None]
    in_blk = jnp.arange(MB_BLOCK, dtype=jnp.int32)
    blk_ids = jnp.arange(nb, dtype=jnp.int32)
    q_chunks = jnp.moveaxis(q.reshape(bsz, h, nq, MB_Q_CHUNK, dh), 2, 0)

    def attend(args):
        c, qc = args
        qf = qc.astype(jnp.float32)
        t = c * MB_Q_CHUNK + jnp.arange(MB_Q_CHUNK, dtype=jnp.int32)
        own = (c * MB_Q_CHUNK) // MB_BLOCK
        gate = jnp.einsum('bhqd,bhnd->bhqn', qf, k_mean)
        gate = jnp.where(blk_ids < own, gate, NEG_INF)
        _, sel = lax.top_k(gate, n_sel)
        valid = sel < own
        k_sel = kb[bi, hi, sel].astype(jnp.float32)
        v_sel = vb[bi, hi, sel].astype(jnp.float32)
        s_sel = jnp.einsum('bhqd,bhqjsd->bhqjs', qf, k_sel) * scale
        dist_sel = (t[:, None, None] - (sel[..., None] * MB_BLOCK + in_blk)).astype(jnp.float32)
        s_sel = s_sel - slopes[..., None, None] * dist_sel
        s_sel = jnp.where(valid[..., None], s_sel, NEG_INF)
        k_own = lax.dynamic_index_in_dim(kb, own, axis=2, keepdims=False).astype(jnp.float32)
        v_own = lax.dynamic_index_in_dim(vb, own, axis=2, keepdims=False).astype(jnp.float32)
        dist_own = t[:, None] - (own * MB_BLOCK + in_blk)[None, :]
        s_own = jnp.einsum('bhqd,bhsd->bhqs', qf, k_own) * scale - slopes[..., None] * dist_own.astype(jnp.float32)
        s_own = jnp.where(dist_own >= 0, s_own, NEG_INF)
        scores = jnp.concatenate([s_sel.reshape(bsz, h, MB_Q_CHUNK, n_sel * MB_BLOCK), s_own], axis=-1)
        p = jax.nn.softmax(scores, axis=-1)
        p_sel = p[..., :n_sel * MB_BLOCK].reshape(bsz, h, MB_Q_CHUNK, n_sel, MB_BLOCK)
        p_own = p[..., n_sel * MB_BLOCK:]
        o = jnp.einsum('bhqjs,bhqjsd->bhqd', p_sel, v_sel) + jnp.einsum('bhqs,bhsd->bhqd', p_own, v_own)
        return o.astype(q.dtype)

    o = lax.map(attend, (jnp.arange(nq, dtype=jnp.int32), q_chunks))
    return jnp.moveaxis(o, 0, 2).reshape(bsz, h, t_len, dh)


def moba_mixer(x, w_in, w_out):
    bsz, t_len, _ = x.shape
    proj = x @ w_in
    heads = lambda y: jnp.transpose(y.reshape(bsz, t_len, MB_HEADS, MB_HEAD_DIM), (0, 2, 1, 3))
    q = heads(proj[..., :MB_WIDTH])
    k = heads(proj[..., MB_WIDTH:2 * MB_WIDTH])
    v = heads(proj[..., 2 * MB_WIDTH:])
    t_pad = -(-t_len // MB_BLOCK) * MB_BLOCK
    pad = ((0, 0), (0, 0), (0, t_pad - t_len), (0, 0))
    o = moba_attention(jnp.pad(q, pad), jnp.pad(k, pad), jnp.pad(v, pad))[:, :, :t_len]
    return jnp.transpose(o, (0, 2, 1, 3)).reshape(bsz, t_len, MB_WIDTH) @ w_out


def clamped_swiglu(h_gate, h_up):
    h_gate = jnp.minimum(h_gate, SWIGLU_LIMIT)
    h_up = jnp.clip(h_up, -SWIGLU_LIMIT, SWIGLU_LIMIT)
    return (h_up + 1.0) * (h_gate * jax.nn.sigmoid(SWIGLU_ALPHA * h_gate))


def moe_ffn(x2d, router_w, router_b, w_gate, b_gate, w_up, b_up, w_down, b_down):
    n_tok, d = x2d.shape
    logits = x2d.astype(jnp.float32) @ router_w.astype(jnp.float32) + router_b.astype(jnp.float32)
    top_vals, top_idx = lax.top_k(logits, TOP_K)
    gates = jax.nn.softmax(top_vals, axis=-1)
    n_assign = n_tok * TOP_K
    flat_e = top_idx.reshape(-1).astype(jnp.int32)
    flat_tok = jnp.repeat(jnp.arange(n_tok, dtype=jnp.int32), TOP_K)
    flat_g = gates.reshape(-1)
    order = jnp.argsort(flat_e)
    sorted_e = flat_e[order]
    counts = jnp.bincount(flat_e, length=N_EXPERTS).astype(jnp.int32)
    starts = jnp.cumsum(counts) - counts
    padded = (counts + MOE_BLOCK - 1) // MOE_BLOCK * MOE_BLOCK
    pends = jnp.cumsum(padded)
    pstarts = pends - padded
    dest = pstarts[sorted_e] + (jnp.arange(n_assign, dtype=jnp.int32) - starts[sorted_e])
    n_blocks = -(-n_assign // MOE_BLOCK) + N_EXPERTS
    rows = jnp.full((n_blocks * MOE_BLOCK,), n_tok, jnp.int32).at[dest].set(flat_tok[order])
    row_gate = jnp.zeros((n_blocks * MOE_BLOCK,), jnp.float32).at[dest].set(flat_g[order])
    block_e = jnp.minimum(jnp.searchsorted(pends, jnp.arange(n_blocks, dtype=jnp.int32) * MOE_BLOCK, side='right'),
                          N_EXPERTS - 1).astype(jnp.int32)
    x_pad = jnp.concatenate([x2d, jnp.zeros((1, d), x2d.dtype)], axis=0)

    def step(acc, inp):
        r, gt, e = inp
        xb = x_pad[r]
        h = clamped_swiglu(xb @ w_gate[e] + b_gate[e], xb @ w_up[e] + b_up[e])
        y = h @ w_down[e] + b_down[e]
        return acc.at[r].add(y.astype(jnp.float32) * gt[:, None]), None

    acc0 = jnp.zeros((n_tok + 1, d), jnp.float32)
    acc, _ = lax.scan(step, acc0, (rows.reshape(n_blocks, MOE_BLOCK), row_gate.reshape(n_blocks, MOE_BLOCK), block_e))
    return acc[:n_tok].astype(x2d.dtype)


def setup_inputs(seed: int = 0) -> dict:
    key = jax.random.key(seed)
    ks = jax.random.split(key, 24)
    n_a = (DEPTH + 1) // 2
    n_b = DEPTH // 2
    conv_ch = 2 * DN_KEY_DIM + DN_VAL_DIM
    nrm = lambda k, shape, scale: jax.random.normal(k, shape, jnp.float32) * scale
    return {
        'x': jax.random.normal(ks[0], (BATCH, SEQ, D_MODEL), jnp.float32),
        'dn_w_in': nrm(ks[1], (n_a, D_MODEL, DN_IN_DIM), D_MODEL ** -0.5),
        'dn_conv_w': nrm(ks[2], (n_a, DN_CONV, conv_ch), DN_CONV ** -0.5),
        'dn_a_log': jnp.log(jax.random.uniform(ks[3], (n_a, DN_V_HEADS), jnp.float32, 1.0, 16.0)),
        'dn_dt_bias': nrm(ks[4], (n_a, DN_V_HEADS), 0.1),
        'dn_norm_w': 1.0 + nrm(ks[5], (n_a, DN_HEAD_DIM), 0.02),
        'dn_w_out': nrm(ks[6], (n_a, DN_VAL_DIM, D_MODEL), DEEPNORM_BETA * DN_VAL_DIM ** -0.5),
        'mb_w_in': nrm(ks[7], (n_b, D_MODEL, 3 * MB_WIDTH), D_MODEL ** -0.5),
        'mb_w_out': nrm(ks[8], (n_b, MB_WIDTH, D_MODEL), DEEPNORM_BETA * MB_WIDTH ** -0.5),
        'ln_g': 1.0 + nrm(ks[9], (DEPTH, 2, D_MODEL), 0.02),
        'ln_b': nrm(ks[10], (DEPTH, 2, D_MODEL), 0.01),
        'router_w': nrm(ks[11], (DEPTH, D_MODEL, N_EXPERTS), D_MODEL ** -0.5),
        'router_b': nrm(ks[12], (DEPTH, N_EXPERTS), 0.01),
        'w_gate': nrm(ks[13], (DEPTH, N_EXPERTS, D_MODEL, D_EXPERT), D_MODEL ** -0.5),
        'b_gate': nrm(ks[14], (DEPTH, N_EXPERTS, D_EXPERT), 0.01),
        'w_up': nrm(ks[15], (DEPTH, N_EXPERTS, D_MODEL, D_EXPERT), D_MODEL ** -0.5),
        'b_up': nrm(ks[16], (DEPTH, N_EXPERTS, D_EXPERT), 0.01),
        'w_down': nrm(ks[17], (DEPTH, N_EXPERTS, D_EXPERT, D_MODEL), DEEPNORM_BETA * D_EXPERT ** -0.5),
        'b_down': nrm(ks[18], (DEPTH, N_EXPERTS, D_MODEL), 0.01),
    }


def reference(x, dn_w_in, dn_conv_w, dn_a_log, dn_dt_bias, dn_norm_w, dn_w_out, mb_w_in, mb_w_out,
              ln_g, ln_b, router_w, router_b, w_gate, b_gate, w_up, b_up, w_down, b_down):
    bsz, t_len, d = x.shape
    for i in range(DEPTH):
        j = i // 2
        if i % 2 == 0:
            h = gated_deltanet_mixer(x, dn_w_in[j], dn_conv_w[j], dn_a_log[j], dn_dt_bias[j], dn_norm_w[j], dn_w_out[j])
        else:
            h = moba_mixer(x, mb_w_in[j], mb_w_out[j])
        x = layer_norm(DEEPNORM_ALPHA * x + h, ln_g[i, 0], ln_b[i, 0])
        f = moe_ffn(x.reshape(bsz * t_len, d), router_w[i], router_b[i], w_gate[i], b_gate[i],
                    w_up[i], b_up[i], w_down[i], b_down[i]).reshape(bsz, t_len, d)
        x = layer_norm(DEEPNORM_ALPHA * x + f, ln_g[i, 1], ln_b[i, 1])
    return x
```

```python
import numpy as np
import concourse.bass as bass
import concourse.mybir as mybir
from concourse.bass_utils import run_bass_kernel_spmd
F32 = mybir.dt.float32; BF16 = mybir.dt.bfloat16
AF = mybir.ActivationFunctionType; ALU = mybir.AluOpType; AX = mybir.AxisListType

class T:
    def __init__(self, ap, name):
        self.ap = ap; self.name = name; self.psum = False
        self.w = None
        self.r = []
    def __getitem__(self, k):
        return V(self, self.ap[k])
class V:
    def __init__(self, t, ap): self.t = t; self.ap = ap
    def __getitem__(self, k): return V(self.t, self.ap[k])
    def re(self, pat, **kw): return V(self.t, self.ap.rearrange(pat, **kw))

class KB:
    def __init__(self, nc):
        self.nc = nc
        self.E = {'pe': nc.tensor, 'act': nc.scalar, 'dve': nc.vector, 'pool': nc.gpsimd, 'sp': nc.sync}
        self.stack = []; self.semstack = []
        self.sem = {}; self.cnt = {}
        for e in ('pe', 'act', 'dve', 'pool'):
            self.sem[e] = self._sem('c_' + e); self.cnt[e] = 0
        self.seen = {e: {} for e in self.E}
        self.nsb = 0
        self.dsems = {}
    def _sem(self, name):
        cm = self.nc.semaphore(name); s = cm.__enter__(); self.semstack.append(cm); return s
    def sb(self, name, shape, dt=F32):
        self.nsb += 1
        cm = self.nc.sbuf_tensor(f"{name}_u{self.nsb}", list(shape), dt); t = cm.__enter__(); self.stack.append(cm)
        return T(t, name)
    def ps(self, name, shape, dt=F32):
        self.nsb += 1
        cm = self.nc.psum_tensor(f"{name}_u{self.nsb}", list(shape), dt); t = cm.__enter__(); self.stack.append(cm)
        r = T(t, name); r.psum = True
        return r
    def dram(self, name, shape, dt, kind="Internal"):
        return T(self.nc.dram_tensor(name, list(shape), dt, kind=kind).ap(), name)
    def close(self):
        for cm in reversed(self.stack): cm.__exit__(None, None, None)
        for cm in reversed(self.semstack): cm.__exit__(None, None, None)
    def _wait(self, eng, deps):
        best = {}
        for (s, v) in deps:
            k = id(s)
            if k not in best or best[k][1] < v: best[k] = (s, v)
        for k, (s, v) in best.items():
            if self.seen[eng].get(k, 0) >= v: continue
            if eng == 'pe' and s is self.sem['pe']: continue
            self.E[eng].wait_ge(s, v); self.seen[eng][k] = v
    def _deps(self, reads, writes):
        d = []
        for v in reads:
            if v.t.w: d.append(v.t.w)
        for v in writes:
            if v.t.w: d.append(v.t.w)
            d.extend(v.t.r)
        return d
    def _done(self, tok, reads, writes):
        for v in reads: v.t.r.append(tok)
        for v in writes: v.t.w = tok; v.t.r = []
    def op(self, eng, fn, reads, writes):
        pr = [v for v in reads if v.t.psum]
        if pr:
            reads = [v for v in reads if not v.t.psum]; writes = list(writes) + pr
        self._wait(eng, self._deps(reads, writes))
        ins = fn(self.E[eng])
        self.cnt[eng] += 1
        ins.then_inc(self.sem[eng], 1)
        self._done((self.sem[eng], self.cnt[eng]), reads, writes)
    def dma(self, q, out, in_, key=None):
        key = key or out.t.name
        if key not in self.dsems: self.dsems[key] = [self._sem('d_' + key), 0]
        ds = self.dsems[key]
        self._wait(q, self._deps([in_], [out]))
        ds[1] += 16
        self.E[q].dma_start(out=out.ap, in_=in_.ap).then_inc(ds[0], 16)
        self._done((ds[0], ds[1]), [in_], [out])
    def wait_all(self, eng, ts):
        d = []
        for t in ts:
            if t.w: d.append(t.w)
            d.extend(t.r)
        self._wait(eng, d)
    def mm(self, out, lhsT, rhs, start=True, stop=True):
        self.op('pe', lambda e: e.matmul(out.ap, lhsT.ap, rhs.ap, start=start, stop=stop), [lhsT, rhs], [out])
    def act(self, out, in_, func, bias=None, scale=1.0, accum=None, eng='act'):
        rd = [in_]; wr = [out]
        kw = {}
        if bias is not None:
            if isinstance(bias, V): rd.append(bias); kw['bias'] = bias.ap
            else: kw['bias'] = bias
        if isinstance(scale, V): rd.append(scale); kw['scale'] = scale.ap
        else: kw['scale'] = scale
        if accum is not None: wr.append(accum); kw['accum_out'] = accum.ap
        self.op('act', lambda e: e.activation(out.ap, in_.ap, func, **kw), rd, wr)
    def ts(self, out, in0, s1, s2, op0, op1=None, eng='dve', accum=None):
        rd = [in0]; wr = [out]
        a1 = s1.ap if isinstance(s1, V) else s1
        a2 = s2.ap if isinstance(s2, V) else s2
        if isinstance(s1, V): rd.append(s1)
        if isinstance(s2, V): rd.append(s2)
        kw = {}
        if accum is not None: wr.append(accum); kw['accum_out'] = accum.ap
        if op1 is None:
            self.op(eng, lambda e: e.tensor_scalar(out.ap, in0.ap, a1, None, op0, **kw), rd, wr)
        else:
            self.op(eng, lambda e: e.tensor_scalar(out.ap, in0.ap, a1, a2, op0, op1, **kw), rd, wr)
    def tt(self, out, in0, in1, op, eng='dve'):
        self.op(eng, lambda e: e.tensor_tensor(out.ap, in0.ap, in1.ap, op), [in0, in1], [out])
    def stt(self, out, in0, s, in1, op0, op1, eng='dve'):
        rd = [in0, in1]
        a = s.ap if isinstance(s, V) else s
        if isinstance(s, V): rd.append(s)
        self.op(eng, lambda e: e.scalar_tensor_tensor(out.ap, in0.ap, a, in1.ap, op0, op1), rd, [out])
    def copy(self, out, in_, eng='dve'):
        if eng == 'act':
            self.op('act', lambda e: e.copy(out.ap, in_.ap), [in_], [out])
        else:
            self.op(eng, lambda e: e.tensor_copy(out.ap, in_.ap), [in_], [out])
    def memset(self, out, val, eng='dve'):
        self.op(eng, lambda e: e.memset(out.ap, val), [], [out])

def _v_bc(self, axis, shape):
    return V(self.t, self.ap.unsqueeze(axis).broadcast_to(list(shape)))
V.bc = _v_bc

ALPHA = 4 ** 0.25
D = 4096; NE = 32; DE = 768; KC = 32; JC = 6

def barrier(k):
    deps = [(k.sem[e], k.cnt[e]) for e in ('pe','act','dve','pool')] + [(s, v) for (s, v) in k.dsems.values()]
    for e in ('pe','act','dve','pool','sp'):
        k._wait(e, [d for d in deps if not (e == 'pe' and d[0] is k.sem['pe'])] if True else deps)

def layer_norm_to_fm(k, acc_s, stats, mv, rstd, ident, psT, gcol, bcol, x1Tf, chunk_cb=None):
    nc = k.nc
    for c in range(8):
        k.op('dve', lambda e, c=c: e.bn_stats(stats[:, c, :].ap, acc_s[:, c*512:(c+1)*512].ap), [acc_s], [stats[:]])
    k.op('dve', lambda e: e.bn_aggr(mv[:].ap, stats[:].ap), [stats[:]], [mv[:]])
    k.act(rstd[:], mv[:, 1:2], AF.Sqrt, bias=1e-5)
    k.op('dve', lambda e: e.reciprocal(rstd[:].ap, rstd[:].ap), [rstd[:]], [rstd[:]])
    k.ts(acc_s, acc_s, mv[:, 0:1], rstd[:, 0:1], ALU.subtract, ALU.mult)
    for kc4 in range(8):
        ps = psT[kc4 % 2]
        for q in range(4):
            kc = kc4 * 4 + q
            k.mm(ps[:, q*128:(q+1)*128], acc_s[:, kc*128:(kc+1)*128], ident[:])
        for q in range(4):
            kc = kc4 * 4 + q
            if kc4 % 2 == 0:
                k.ts(x1Tf[:, kc, :], ps[:, q*128:(q+1)*128], gcol[:, kc:kc+1], bcol[:, kc:kc+1], ALU.mult, ALU.add)
            else:
                k.act(x1Tf[:, kc, :], ps[:, q*128:(q+1)*128], AF.Identity, bias=bcol[:, kc:kc+1], scale=gcol[:, kc:kc+1])

STAGE = 99
def post_block(k, NT, TT, oT_src, xres, x_out, xT_out, wo, lng, lnb, rw, rb, wg, wu, wd, bg, bu, bd, consts, ne=NE, selmask=None):
    NS = TT // 128
    ident, alphaI = consts['ident'], consts['alphaI']
    acc = [k.sb(f"acc{s}", [128, D]) for s in range(NS)]
    xT = k.sb("xT", [128, KC, TT], BF16)
    ring = [k.sb(f"ring{i}", [128, KC, 128], BF16) for i in range(4)]
    wdr = [k.sb(f"wdr{i}", [128, JC, 512], BF16) for i in range(2)]
    hT = k.sb("hT", [128, JC, TT], BF16)
    g_sb = k.sb("g_sb", [128, TT]); sg_sb = k.sb("sg_sb", [128, TT]); u_sb = k.sb("u_sb", [128, TT])
    xin = k.sb("xin", [128, D])
    xinv = V(xin, xin.ap.rearrange("p (k t) -> p k t", k=KC))
    stats = k.sb("stats", [128, 8, 6]); mv = k.sb("mv", [128, 2]); rstd = k.sb("rstd", [128, 1])
    gcol = k.sb("gcol", [128, 2, KC]); bcol = k.sb("bcol", [128, 2, KC])
    rw_sb = k.sb("rw_sb", [128, KC, NE]); rb_sb = k.sb("rb_sb", [128, NE])
    bg_sb = k.sb("bg_sb", [128, ne, JC]); bu_sb = k.sb("bu_sb", [128, ne, JC]); bd_sb = k.sb("bd_sb", [NE, D])
    lg = k.sb("lg", [128, NE]); m8 = k.sb("m8", [128, 8]); negm = k.sb("negm", [128, 1]); msk = k.sb("msk", [128, NE])
    ssum = k.sb("ssum", [128, 1]); gw = [k.sb(f"gw{s}", [128, NE]) for s in range(NS)]
    gwT = k.sb("gwT", [NE, 128])
    bf_out = k.sb("bf_out", [128, KC, 128], BF16)
    psA = [k.ps(f"psA{i}", [128, 512]) for i in range(2)]
    psB = [k.ps(f"psB{i}", [128, 512]) for i in range(2)]
    psC = [k.ps(f"psC{i}", [128, 512]) for i in range(2)]
    psT = [k.ps(f"psT{i}", [128, 512]) for i in range(2)]
    k.dma('sp', gcol[:], lng[:].re("l p k -> p l k")); k.dma('sp', bcol[:], lnb[:].re("l p k -> p l k"))
    k.dma('sp', rw_sb[:], rw[:]); k.dma('sp', rb_sb[:], rb[:])
    k.dma('sp', bg_sb[:], bg[:]); k.dma('sp', bu_sb[:], bu[:]); k.dma('sp', bd_sb[:], bd[:])
    rix = [0]
    def ring_load(src):
        t = ring[rix[0] % 4]; rix[0] += 1
        k.dma('pool', t[:], src); return t
    wix = [0]
    for t0 in range(0, NT, TT):
        if selmask is None:
            k.dma('sp', xT[:], oT_src(t0, TT))
        else:
            stg = V(bf_out, bf_out.ap.rearrange("p k t -> p (k t)").rearrange("p (a b) -> p a b", a=8))
            for kg in range(4):
                for j in range(8):
                    k.dma('sp', stg, oT_src(j, kg, t0, TT))
                    if j == 0: k.ts(xT[:, kg*8:(kg+1)*8, :], stg, selmask[:, 0:1], None, ALU.mult)
                    else: k.stt(xT[:, kg*8:(kg+1)*8, :], stg, selmask[:, j:j+1], xT[:, kg*8:(kg+1)*8, :], ALU.mult, ALU.add)
        if STAGE==0: return
        for blk in range(32):
            w = ring_load(wo[blk])
            ps = psC[blk % 2]
            for s in range(NS):
                for kc in range(KC):
                    k.mm(ps[:, s*128:(s+1)*128], xT[:, kc, s*128:(s+1)*128], w[:, kc, :], start=(kc == 0), stop=(kc == KC-1))
            for s in range(NS):
                if blk % 2 == 0: k.copy(acc[s][:, blk*128:(blk+1)*128], ps[:, s*128:(s+1)*128])
                else: k.act(acc[s][:, blk*128:(blk+1)*128], ps[:, s*128:(s+1)*128], AF.Copy)
        if STAGE==1: return
        for s in range(NS):
            k.dma('sp', xin[:], xres[t0+s*128:t0+(s+1)*128, :])
            k.stt(acc[s][:], xin[:], ALPHA, acc[s][:], ALU.mult, ALU.add)
            layer_norm_to_fm(k, acc[s][:], stats, mv, rstd, ident, psT, gcol[:, 0, :], bcol[:, 0, :], xinv)
            if STAGE==2: return
            k.copy(xT[:, :, s*128:(s+1)*128], xinv[:])
            pr = psC[0]
            for kc in range(KC):
                k.mm(pr[:, 0:NE], xinv[:, kc, :], rw_sb[:, kc, :], start=(kc == 0), stop=(kc == KC-1))
            k.tt(lg[:], pr[:, 0:NE], rb_sb[:], ALU.add)
            k.op('dve', lambda e: e.max(m8[:].ap, lg[:].ap), [lg[:]], [m8[:]])
            k.ts(negm[:], m8[:, 0:1], -1.0, None, ALU.mult)
            k.ts(msk[:], lg[:], m8[:, 3:4], None, ALU.is_ge)
            k.act(lg[:], lg[:], AF.Exp, bias=negm[:, 0:1])
            k.tt(lg[:], lg[:], msk[:], ALU.mult)
            k.op('dve', lambda e: e.reduce_sum(ssum[:].ap, lg[:].ap, AX.X), [lg[:]], [ssum[:]])
            k.op('dve', lambda e: e.reciprocal(ssum[:].ap, ssum[:].ap), [ssum[:]], [ssum[:]])
            k.ts(gw[s][:], lg[:], ssum[:, 0:1], None, ALU.mult)
            if STAGE==3: return
            k.mm(psT[0][0:NE, 0:128], gw[s][:], ident[:])
            k.copy(gwT[:], psT[0][0:NE, 0:128])
            for b4 in range(8):
                ps = psC[b4 % 2]
                for q in range(4):
                    kc = b4*4+q
                    k.mm(ps[:, q*128:(q+1)*128], xinv[:, kc, :], alphaI[:], start=True, stop=False)
                    k.mm(ps[:, q*128:(q+1)*128], gwT[:], bd_sb[:, kc*128:(kc+1)*128], start=False, stop=True)
                if b4 % 2 == 0: k.copy(acc[s][:, b4*512:(b4+1)*512], ps[:])
                else: k.act(acc[s][:, b4*512:(b4+1)*512], ps[:], AF.Copy)
        if STAGE==4: return
        for e in range(ne):
            for j in range(JC):
                wgt = ring_load(wg[e, j]); wut = ring_load(wu[e, j])
                pg = psA[j % 2]; pu = psB[j % 2]
                for kc in range(KC):
                    k.mm(pg[:, 0:TT], wgt[:, kc, :], xT[:, kc, :], start=(kc == 0), stop=(kc == KC-1))
                for kc in range(KC):
                    k.mm(pu[:, 0:TT], wut[:, kc, :], xT[:, kc, :], start=(kc == 0), stop=(kc == KC-1))
                k.ts(g_sb[:], pg[:, 0:TT], bg_sb[:, e, j:j+1], 7.0, ALU.add, ALU.min)
                k.act(sg_sb[:], g_sb[:], AF.Sigmoid, scale=1.702)
                k.ts(u_sb[:], pu[:, 0:TT], bu_sb[:, e, j:j+1], 7.0, ALU.add, ALU.min)
                k.ts(u_sb[:], u_sb[:], -7.0, 1.0, ALU.max, ALU.add)
                k.tt(g_sb[:], g_sb[:], sg_sb[:], ALU.mult)
                k.tt(hT[:, j, :], u_sb[:], g_sb[:], ALU.mult)
            for cb in range(8):
                wdt = wdr[wix[0] % 2]; wix[0] += 1
                k.dma('pool', wdt[:], wd[e, cb])
                for s in range(NS):
                    ps = psC[(cb*NS+s) % 2]
                    for j in range(JC):
                        k.mm(ps[:], hT[:, j, s*128:(s+1)*128], wdt[:, j, :], start=(j == 0), stop=(j == JC-1))
                    k.stt(acc[s][:, cb*512:(cb+1)*512], ps[:], gw[s][:, e:e+1], acc[s][:, cb*512:(cb+1)*512], ALU.mult, ALU.add)
        if STAGE==5: return
        for s in range(NS):
            k.stt(acc[s][:], acc[s][:], 1.0, acc[s][:], ALU.mult, ALU.bypass) if False else None
            layer_norm_to_fm(k, acc[s][:], stats, mv, rstd, ident, psT, gcol[:, 1, :], bcol[:, 1, :], xinv)
            if xT_out is not None:
                k.copy(bf_out[:], xinv[:])
                k.dma('sp', xT_out(t0 + s*128, 128), bf_out[:])
            for b4 in range(8):
                ps = psC[b4 % 2]
                for q in range(4):
                    kc = b4*4+q
                    k.mm(ps[:, q*128:(q+1)*128], xinv[:, kc, :], ident[:])
                if b4 % 2 == 0: k.copy(acc[s][:, b4*512:(b4+1)*512], ps[:])
                else: k.act(acc[s][:, b4*512:(b4+1)*512], ps[:], AF.Copy)
            k.dma('sp', x_out[t0+s*128:t0+(s+1)*128, :], acc[s][:])

KC = 32
def deltanet(k, NTOK, TT, xload, w, wba, convw, alog, dtb, normw, oT_dst, consts, tile_cb=None):
    ident = consts['ident']
    NCH = TT // 64
    ring = [k.sb(f"dring{i}", [128, KC, 128], BF16) for i in range(3)]
    xT = [k.sb(f"dxT{i}", [128, KC, TT], BF16) for i in range(1)]
    wba_sb = k.sb("wba_sb", [128, KC, 16], BF16)
    cw = k.sb("cw", [128, 16, 4])
    raw = [k.sb(f"raw{c}", [128, 3 + TT]) for c in range(16)]
    zfm = k.sb("zfm", [128, 8, TT])
    cv = k.sb("cv", [128, TT]); sq = k.sb("sq", [128, TT]); rs = sq
    sgm = k.sb("sgm", [128, TT])
    qn = k.sb("qn", [128, 4, TT]); kn = k.sb("kn", [128, 4, TT]); vf = k.sb("vf", [128, 8, TT])
    ones = k.sb("ones", [128, 128]); k.memset(ones[:], 1.0)
    tri = k.sb("tri", [64, 64]); negU = k.sb("negU", [64, 64]); negLs = k.sb("negLs", [64, 64])
    k.memset(tri[:], 1.0, eng='pool'); k.memset(negU[:], 0.0, eng='pool'); k.memset(negLs[:], 0.0, eng='pool')
    k.op('pool', lambda e: e.affine_select(tri[:].ap, tri[:].ap, [[1, 64]], ALU.is_ge, 0.0, base=0, channel_multiplier=-1), [], [tri[:]])
    k.op('pool', lambda e: e.affine_select(negU[:].ap, negU[:].ap, [[1, 64]], ALU.is_ge, -1e4, base=0, channel_multiplier=-1), [], [negU[:]])
    k.op('pool', lambda e: e.affine_select(negLs[:].ap, negLs[:].ap, [[-1, 64]], ALU.is_gt, -1e4, base=0, channel_multiplier=1), [], [negLs[:]])
    negA = k.sb("negA", [64, 8]); dtb_sb = k.sb("dtb_sb", [64, 8]); nw = k.sb("nw", [64, 128])
    k.dma('sp', negA[:], alog[:]); k.dma('sp', dtb_sb[:], dtb[:]); k.dma('sp', nw[:], normw[:])
    k.act(negA[:], negA[:], AF.Exp); k.ts(negA[:], negA[:], -1.0, None, ALU.mult)
    k.dma('pool', wba_sb[:], wba[:]); k.dma('sp', cw[:], convw[:])
    for c in range(16): k.memset(raw[c][:, 0:3], 0.0)
    S = k.sb("S", [128, 8, 128]); k.memset(S[:], 0.0)
    ba = k.sb("ba", [64, 16]); beta = k.sb("beta", [64, 8]); nbeta = k.sb("nbeta", [64, 8]); graw = k.sb("graw", [64, 8])
    gcol = k.sb("gcolc", [64, 8]); gt = k.sb("gt", [128, 8]); dcol = k.sb("dcol", [64, 8])
    R1 = k.sb("R1", [64, 8, 64]); R2 = k.sb("R2", [64, 8, 64])
    EG = k.sb("EG", [128, 8, 64]); BE = k.sb("BE", [128, 8, 64])
    tmpd = R1; E = R2; ET = k.sb("ET", [64, 8, 64])
    Xb = [k.sb(f"Xb{i}", [64, 8, 64]) for i in range(2)]; XTb = [k.sb(f"XTb{i}", [64, 8, 64]) for i in range(2)]
    Y = [k.sb(f"Y{i}", [64, 8, 64]) for i in range(2)]
    inT = k.sb("inT", [64, 8, 64])
    KbgT = k.sb("KbgT", [128, 8, 64]); QgT = k.sb("QgT", [128, 8, 64])
    Kd = k.sb("Kd", [64, 8, 128]); Vb = k.sb("Vb", [64, 8, 128]); sz = Kd
    rv = Vb; vn = k.sb("vn", [64, 8, 128]); og = k.sb("og", [64, 8, 128])
    ss = k.sb("ss", [64, 8])
    ogT = k.sb("ogT", [128, 8, TT], BF16)
    P = [k.ps(f"dP{i}", [128, 512]) for i in range(6)]
    W0 = k.ps("dW0", [128, 1024])
    I64 = ident[0:64, 0:64]
    rix = [0]
    ntile = NTOK // TT
    def load_x(ti):
        xload(xT[0], ti * TT, TT)
    load_x(0)
    for ti in range(ntile):
        t0 = ti * TT
        x = xT[0]
        if ti > 0: load_x(ti)
        if tile_cb is not None: tile_cb(ti)
        for c in range(24):
            wt = ring[rix[0] % 3]; rix[0] += 1
            k.dma('pool', wt[:], w[c])
            ps = P[c % 2]
            for kc in range(KC):
                k.mm(ps[:, 0:TT], wt[:, kc, :], x[:, kc, :], start=(kc == 0), stop=(kc == KC - 1))
            if c < 16:
                if c % 2 == 0: k.copy(raw[c][:, 3:3 + TT], ps[:, 0:TT])
                else: k.act(raw[c][:, 3:3 + TT], ps[:, 0:TT], AF.Copy)
            else:
                if c % 2 == 0: k.copy(zfm[:, c - 16, :], ps[:, 0:TT])
                else: k.act(zfm[:, c - 16, :], ps[:, 0:TT], AF.Copy)
        for c in range(16):
            r = raw[c]
            k.ts(cv[:], r[:, 0:TT], cw[:, c, 0:1], None, ALU.mult)
            for j in range(1, 4):
                k.stt(cv[:], r[:, j:j + TT], cw[:, c, j:j + 1], cv[:], ALU.mult, ALU.add)
            k.copy(r[:, 0:3], r[:, TT:TT + 3], eng='pool')
            k.act(sgm[:], cv[:], AF.Sigmoid)
            if c < 8:
                k.tt(cv[:], cv[:], sgm[:], ALU.mult)
                k.tt(sq[:], cv[:], cv[:], ALU.mult)
                pn = P[2 + (c % 2)]
                k.mm(pn[:, 0:TT], ones[:], sq[:])
                k.act(rs[:], pn[:, 0:TT], AF.Sqrt, bias=1e-6)
                k.op('dve', lambda e: e.reciprocal(rs[:].ap, rs[:].ap), [rs[:]], [rs[:]])
                if c < 4: k.stt(qn[:, c, :], cv[:], 128 ** -0.5, rs[:], ALU.mult, ALU.mult)
                else: k.tt(kn[:, c - 4, :], cv[:], rs[:], ALU.mult)
            else:
                k.tt(vf[:, c - 8, :], cv[:], sgm[:], ALU.mult)
        for ch in range(NCH):
            cs = slice(ch * 64, (ch + 1) * 64)
            for kc in range(KC):
                k.mm(P[0][0:64, 0:16], x[:, kc, cs], wba_sb[:, kc, :], start=(kc == 0), stop=(kc == KC - 1))
            k.copy(ba[:], P[0][0:64, 0:16])
            k.act(beta[:], ba[:, 0:8], AF.Sigmoid)
            k.ts(nbeta[:], beta[:], -1.0, None, ALU.mult)
            k.tt(graw[:], ba[:, 8:16], dtb_sb[:], ALU.add)
            k.act(graw[:], graw[:], AF.Exp)
            k.act(graw[:], graw[:], AF.Ln, bias=1.0)
            k.tt(graw[:], graw[:], negA[:], ALU.mult)
            k.mm(P[0][0:64, 16:24], tri[:], graw[:])
            k.mm(P[0][:, 24:32], ones[0:64, :], graw[:])
            k.copy(gcol[:], P[0][0:64, 16:24])
            k.act(gt[:], P[0][:, 24:32], AF.Exp)
            k.tt(dcol[:], P[0][0:64, 24:32], gcol[:], ALU.subtract)
            k.act(dcol[:], dcol[:], AF.Exp)
            k.tt(R1[:], graw[:].bc(2, [64, 8, 64]), tri[:].bc(1, [64, 8, 64]), ALU.mult)
            k.tt(R2[:], beta[:].bc(2, [64, 8, 64]), I64.bc(1, [64, 8, 64]), ALU.mult)
            k.mm(P[1][:], ones[0:64, :], R1[:].re("p h i -> p (h i)"))
            k.mm(P[2][:], ones[0:64, :], R2[:].re("p h i -> p (h i)"))
            k.act(EG[:].re("p h i -> p (h i)"), P[1][:], AF.Exp)
            k.tt(BE[:].re("p h i -> p (h i)"), P[2][:], EG[:].re("p h i -> p (h i)"), ALU.mult)
            k.tt(tmpd[:], P[1][0:64, :].re("p (h i) -> p h i", h=8), gcol[:].bc(2, [64, 8, 64]), ALU.subtract)
            k.tt(E[:], tmpd[:], negU[:].bc(1, [64, 8, 64]), ALU.add)
            k.act(E[:], E[:], AF.Exp)
            k.stt(ET[:], tmpd[:], -1.0, negLs[:].bc(1, [64, 8, 64]), ALU.mult, ALU.add)
            k.act(ET[:], ET[:], AF.Exp)
            for kh in range(4):
                k.mm(P[3][0:64, kh * 64:(kh + 1) * 64], kn[:, kh, cs], kn[:, kh, cs])
                k.mm(P[3][0:64, 256 + kh * 64:256 + (kh + 1) * 64], kn[:, kh, cs], qn[:, kh, cs])
            Gv = P[3][0:64, 0:256].re("p (a i) -> p a i", a=4).bc(2, [64, 4, 2, 64])
            KQv = P[3][0:64, 256:512].re("p (a i) -> p a i", a=4).bc(2, [64, 4, 2, 64])
            X0T = XTb[0]
            k.tt(X0T[:].re("p (a b) i -> p a b i", a=4), Gv, ET[:].re("p (a b) i -> p a b i", a=4), ALU.mult)
            k.tt(X0T[:], X0T[:], nbeta[:].bc(2, [64, 8, 64]), ALU.mult)
            k.tt(inT[:].re("p (a b) i -> p a b i", a=4), KQv, E[:].re("p (a b) i -> p a b i", a=4), ALU.mult)
            for h in range(8):
                k.mm(P[4][0:64, h * 64:(h + 1) * 64], X0T[:, h, :], I64)
            k.copy(Xb[0][:].re("p h i -> p (h i)"), P[4][0:64, :])
            k.tt(Y[0][:], Xb[0][:], I64.bc(1, [64, 8, 64]), ALU.add)
            cur = 0
            for lvl in range(1, 6):
                nx = 1 - cur
                for h in range(8):
                    k.mm(P[5][0:64, h * 64:(h + 1) * 64], Xb[cur][:, h, :], XTb[cur][:, h, :])
                k.act(XTb[nx][:].re("p h i -> p (h i)"), P[5][0:64, :], AF.Copy)
                if lvl < 5:
                    for h in range(8):
                        k.mm(P[4][0:64, h * 64:(h + 1) * 64], XTb[cur][:, h, :], Xb[cur][:, h, :])
                    k.copy(Xb[nx][:].re("p h i -> p (h i)"), P[4][0:64, :])
                for h in range(8):
                    k.mm(P[2][0:64, h * 64:(h + 1) * 64], XTb[nx][:, h, :], Y[cur][:, h, :])
                k.tt(Y[nx][:].re("p h i -> p (h i)"), Y[cur][:].re("p h i -> p (h i)"), P[2][0:64, :], ALU.add)
                cur = nx
            TTm = Y[cur]
            k.tt(KbgT[:].re("p (a b) i -> p a b i", a=4), kn[:, :, cs].bc(2, [128, 4, 2, 64]), BE[:].re("p (a b) i -> p a b i", a=4), ALU.mult)
            k.tt(QgT[:].re("p (a b) i -> p a b i", a=4), qn[:, :, cs].bc(2, [128, 4, 2, 64]), EG[:].re("p (a b) i -> p a b i", a=4), ALU.mult)
            for kh in range(4):
                k.mm(P[3][0:64, kh * 128:(kh + 1) * 128], kn[:, kh, cs], ident[:])
            k.tt(Kd[:].re("p (a b) d -> p a b d", a=4), P[3][0:64, :].re("p (a d) -> p a d", a=4).bc(2, [64, 4, 2, 128]),
                 dcol[:].re("p (a b) -> p a b", a=4).bc(3, [64, 4, 2, 128]), ALU.mult)
            for h in range(8):
                k.mm(W0[0:64, h * 128:(h + 1) * 128], vf[:, h, cs], ident[:])
            k.tt(Vb[:], W0[0:64, :].re("p (h d) -> p h d", h=8), beta[:].bc(2, [64, 8, 128]), ALU.mult)
            for h in range(8):
                k.mm(W0[0:64, h * 128:(h + 1) * 128], KbgT[:, h, :], S[:, h, :])
            k.tt(rv[:], Vb[:], W0[0:64, :].re("p (h d) -> p h d", h=8), ALU.subtract)
            for h in range(8):
                k.mm(W0[0:64, h * 128:(h + 1) * 128], TTm[:, h, :], rv[:, h, :])
            k.copy(vn[:].re("p h d -> p (h d)"), W0[0:64, :])
            for h in range(8):
                k.mm(W0[0:64, h * 128:(h + 1) * 128], QgT[:, h, :], S[:, h, :], start=True, stop=False)
                k.mm(W0[0:64, h * 128:(h + 1) * 128], inT[:, h, :], vn[:, h, :], start=False, stop=True)
            k.act(og[:].re("p h d -> p (h d)"), W0[0:64, :], AF.Copy)
            for h in range(8):
                k.mm(W0[:, h * 128:(h + 1) * 128], Kd[:, h, :], vn[:, h, :])
            k.tt(S[:], S[:], gt[:].bc(2, [128, 8, 128]), ALU.mult)
            k.tt(S[:].re("p h d -> p (h d)"), S[:].re("p h d -> p (h d)"), W0[:, :], ALU.add)
            for h in range(8):
                k.mm(W0[0:64, h * 128:(h + 1) * 128], zfm[:, h, cs], ident[:])
            k.act(sz[:].re("p h d -> p (h d)"), W0[0:64, :], AF.Sigmoid)
            k.tt(sz[:].re("p h d -> p (h d)"), sz[:].re("p h d -> p (h d)"), W0[0:64, :], ALU.mult)
            k.tt(rv[:], og[:], og[:], ALU.mult)
            k.op('dve', lambda e: e.reduce_sum(ss[:].ap, rv[:].ap, AX.X), [rv[:]], [ss[:]])
            k.act(ss[:], ss[:], AF.Sqrt, bias=1e-6, scale=1.0 / 128)
            k.op('dve', lambda e: e.reciprocal(ss[:].ap, ss[:].ap), [ss[:]], [ss[:]])
            k.tt(og[:], og[:], ss[:].bc(2, [64, 8, 128]), ALU.mult)
            k.tt(og[:], og[:], nw[:].bc(1, [64, 8, 128]), ALU.mult)
            k.tt(og[:], og[:], sz[:], ALU.mult)
            for h in range(8):
                k.mm(P[5][:, h * 64:(h + 1) * 64], og[:, h, :], I64)
            k.copy(ogT[:, :, cs], P[5][:, :].re("p (h i) -> p h i", h=8))
        k.dma('sp', oT_dst(t0, TT), ogT[:])

KC = 32
SCALE = 128 ** -0.5
BIG = 30000.0
def moba_consts_host(head_ids):
    nh = len(head_ids)
    slopes = np.array([2.0 ** (-8.0 * (h + 1) / 32) for h in head_ids], np.float64)
    p = np.arange(128)[:, None, None]
    dl = np.arange(-3, 64)[None, None, :]
    bcol = (slopes[None, :, None] * (p - 128 * dl)).astype(np.float32)
    i = np.arange(512)[None, :]
    val = (-slopes[:, None] * i / SCALE)
    import ml_dtypes
    hi = val.astype(np.float32).astype(ml_dtypes.bfloat16).astype(np.float64)
    lo = val - hi
    alq = np.stack([hi, lo], 0).astype(np.float32)
    sel = np.zeros((34, 32, 128), np.float32)
    for j in range(32): sel[j, j, :] = 1.0
    sel[32:34] = 1.0
    caus = np.zeros((128, 4, 512), np.float32)
    for off in range(4):
        caus[:, off, :] = np.where((off * 128 + np.arange(128)[:, None]) <= np.arange(512)[None, :], 0.0, -BIG)
    return {"mb_bcol": bcol, "mb_alq": alq, "mb_sel": sel, "mb_caus": caus}

def moba(k, NTOK, NH, xload, w, oT_dst, cd, consts):
    ident = consts['ident']
    TT = 512; NT = NTOK // TT; NBLK = NTOK // 256; NKC = NTOK // 128
    ring = [k.sb(f"mring{i}", [128, KC, 128], BF16) for i in range(4)]
    xT = k.sb("mxT", [128, KC, TT], BF16)
    QT = k.sb("QT", [128, NTOK], BF16); KTt = k.sb("KTt", [128, NTOK], BF16)
    Vp = k.sb("Vp", [128, NKC, 129], BF16)
    k.memset(Vp[:, :, 128:129], 1.0)
    sel = k.sb("sel", [34, 32, 128], BF16); caus = k.sb("caus", [128, 4, 512], BF16)
    bcol = k.sb("bcol_m", [128, NH, 67]); identb = k.sb("identb", [128, 128], BF16)
    k.dma('pool', sel[:], cd['mb_sel'][:]); k.dma('pool', caus[:], cd['mb_caus'][:]); k.dma('sp', bcol[:], cd['mb_bcol'][:])
    k.copy(identb[:], ident[:])
    aug = [k.sb(f"aug{i}", [34, 512], BF16) for i in range(2)]
    kms = k.sb("kms", [128, 32]); kmean = k.sb("kmean", [128, 32], BF16)
    mq = k.sb("mq", [128, 32]); g8 = k.sb("g8", [128, 32]); m8 = k.sb("m8m", [128, 8])
    PT = [k.sb(f"PT{i}", [128, 512], BF16) for i in range(2)]
    on = [k.sb(f"on{i}", [128, 128]) for i in range(2)]; rec = k.sb("rec", [128, 1])
    oTs = k.sb("oTs", [128, 512], BF16)
    PS = [k.ps(f"mPS{i}", [128, 512]) for i in range(2)]
    PO = [k.ps(f"mPO{i}", [128, 512]) for i in range(4)]
    PM = [k.ps(f"mPM{i}", [128, 512]) for i in range(2)]
    rix = [0]
    def rl(src):
        t = ring[rix[0] % 4]; rix[0] += 1
        k.dma('pool', t[:], src); return t
    for h in range(NH):
        wq = rl(w[h, 0]); wk = rl(w[h, 1]); wv = rl(w[h, 2])
        for a in range(2):
            k.dma('pool', aug[a][32:34, :], cd['mb_alq'][:, h, :])
        for ti in range(NT):
            xload(xT, ti * TT, TT)
            for kc in range(KC):
                k.mm(PS[0][:], wq[:, kc, :], xT[:, kc, :], start=(kc == 0), stop=(kc == KC - 1))
            k.copy(QT[:, ti * TT:(ti + 1) * TT], PS[0][:])
            for kc in range(KC):
                k.mm(PS[1][:], wk[:, kc, :], xT[:, kc, :], start=(kc == 0), stop=(kc == KC - 1))
            k.act(KTt[:, ti * TT:(ti + 1) * TT], PS[1][:], AF.Copy)
            for s in range(4):
                for kc in range(KC):
                    k.mm(PM[0][:, s * 128:(s + 1) * 128], xT[:, kc, s * 128:(s + 1) * 128], wv[:, kc, :], start=(kc == 0), stop=(kc == KC - 1))
            k.copy(Vp[:, ti * 4:(ti + 1) * 4, 0:128], PM[0][:].re("p (s d) -> p s d", s=4))
        k.op('dve', lambda e: e.reduce_sum(kms[:, 0:NBLK].ap, KTt[:].re("p (b t) -> p b t", t=256).ap, AX.X), [KTt[:]], [kms[:]])
        if NBLK < 32: k.memset(kms[:, NBLK:32], 0.0)
        k.ts(kmean[:], kms[:], 1.0 / 256, None, ALU.mult)
        for qt in range(NT):
            ag = aug[qt % 2]
            for s4 in range(4):
                sq = qt * 4 + s4; own = sq // 2
                k.mm(PM[1][:, 0:32], QT[:, sq * 128:(sq + 1) * 128], kmean[:])
                k.memset(mq[:], -BIG)
                if own >= 1:
                    if own <= 3:
                        k.memset(mq[:, 0:own], 0.0)
                    else:
                        if own < 8:
                            k.memset(g8[:, 0:8], -1e30)
                            k.copy(g8[:, 0:own], PM[1][:, 0:own])
                            k.op('dve', lambda e: e.max(m8[:].ap, g8[:, 0:8].ap), [g8[:]], [m8[:]])
                        else:
                            k.copy(g8[:, 0:own], PM[1][:, 0:own])
                            k.op('dve', lambda e, own=own: e.max(m8[:].ap, g8[:, 0:own].ap), [g8[:]], [m8[:]])
                        k.ts(mq[:, 0:own], g8[:, 0:own], m8[:, 2:3], None, ALU.is_ge)
                        k.ts(mq[:, 0:own], mq[:, 0:own], -1.0, BIG, ALU.add, ALU.mult)
                k.memset(mq[:, own:own + 1], 0.0)
                k.mm(PM[1][0:32, 128:256], mq[:], ident[:])
                k.copy(ag[0:32, s4 * 128:(s4 + 1) * 128], PM[1][0:32, 128:256])
            nkc = 4 * qt + 4
            def emit_S(kc):
                ps = PS[kc % 2]
                diag = kc >= 4 * qt
                k.mm(ps[:], KTt[:, kc * 128:(kc + 1) * 128], QT[:, qt * TT:(qt + 1) * TT], start=True, stop=False)
                k.mm(ps[:], sel[:, kc // 2, :], ag[:], start=False, stop=not diag)
                if diag:
                    k.mm(ps[:], identb[:], caus[:, kc - 4 * qt, :], start=False, stop=True)
            emit_S(0)
            for kc in range(nkc):
                if kc + 1 < nkc: emit_S(kc + 1)
                pt = PT[kc % 2]
                di = (4 * qt - kc) + 3
                k.act(pt[:], PS[kc % 2][:], AF.Exp, bias=bcol[:, h, di:di + 1], scale=SCALE)
                for s4 in range(4):
                    k.mm(PO[s4][:, 0:129], pt[:, s4 * 128:(s4 + 1) * 128], Vp[:, kc, :], start=(kc == 0), stop=(kc == nkc - 1))
            for s4 in range(4):
                o_ = on[s4 % 2]
                k.op('dve', lambda e, s4=s4: e.reciprocal(rec[:].ap, PO[s4][:, 128:129].ap), [PO[s4][:]], [rec[:]])
                k.ts(o_[:], PO[s4][:, 0:128], rec[:, 0:1], None, ALU.mult)
                k.mm(PM[0][:, s4 * 128:(s4 + 1) * 128], o_[:], ident[:])
            k.copy(oTs[:], PM[0][:])
            k.dma('sp', oT_dst(h, qt * TT, TT), oTs[:])


NB = 2; SEQ = 8192; NTC = 2048; DM = 4096
_KSTOP = 99

def _collective(k, groups, in_t, out_t, key, block=False):
    if key not in k.dsems: k.dsems[key] = [k._sem('c_' + key), 0]
    ds = k.dsems[key]
    deps = k._deps([in_t[:]], [out_t[:]])
    if getattr(k, 'last_cc', None): deps.append(k.last_cc)
    k._wait('pool', deps)
    ds[1] += 1
    k.nc.gpsimd.collective_compute("AllGather", ALU.bypass, replica_groups=groups, ins=[in_t.ap], outs=[out_t.ap]).then_inc(ds[0], 1)
    k._done((ds[0], ds[1]), [in_t[:]], [out_t[:]])
    k.last_cc = (ds[0], ds[1])
    if block: k._wait('pool', [(ds[0], ds[1])])

def build_program(kstop=99):
    nc = bass.Bass("TRN2", target_bir_lowering=False)
    k = KB(nc)
    EI = "ExternalInput"
    G4 = [[0, 1, 2, 3], [4, 5, 6, 7]]; G8 = [list(range(8))]
    xT_loc = k.dram("xT_loc", [DM, NTC], F32, kind=EI)
    xres = k.dram("xres", [NTC, DM], F32, kind=EI)
    selm_d = k.dram("selm", [128, 8], F32, kind=EI); selb_d = k.dram("selb", [128, 2], F32, kind=EI)
    ident_d = k.dram("ident_d", [128, 128], F32, kind=EI)
    dn_w = k.dram("dn_w", [24, 128, 32, 128], F32, kind=EI); dn_wba = k.dram("dn_wba", [128, 32, 16], F32, kind=EI)
    dn_cw = k.dram("dn_cw", [128, 16, 4], F32, kind=EI); dn_al = k.dram("dn_al", [64, 8], F32, kind=EI)
    dn_dtb = k.dram("dn_dtb", [64, 8], F32, kind=EI); dn_nw = k.dram("dn_nw", [64, 128], F32, kind=EI)
    mb_w = k.dram("mb_w", [8, 3, 128, 32, 128], F32, kind=EI)
    cd = {"mb_bcol": k.dram("mb_bcol", [128, 8, 67], F32, kind=EI), "mb_alq": k.dram("mb_alq", [2, 8, 512], F32, kind=EI),
          "mb_sel": k.dram("mb_sel", [34, 32, 128], F32, kind=EI), "mb_caus": k.dram("mb_caus", [128, 4, 512], F32, kind=EI)}
    lng = k.dram("lng", [2, 2, 128, 32], F32, kind=EI); lnb = k.dram("lnb", [2, 2, 128, 32], F32, kind=EI)
    rw = k.dram("rw", [2, 128, 32, 32], F32, kind=EI); rb = k.dram("rb", [2, 128, 32], F32, kind=EI)
    bg = k.dram("bg", [2, 128, 32, 6], F32, kind=EI); bu = k.dram("bu", [2, 128, 32, 6], F32, kind=EI)
    bd = k.dram("bd", [2, 32, DM], F32, kind=EI)
    wo_loc = k.dram("wo_loc", [2, 4 * 128, 4096], F32, kind=EI)
    wg_loc = k.dram("wg_loc", [2, 4 * 6 * 128, 4096], F32, kind=EI)
    wu_loc = k.dram("wu_loc", [2, 4 * 6 * 128, 4096], F32, kind=EI)
    wd_loc = k.dram("wd_loc", [2, 4 * 8 * 128, 3072], F32, kind=EI)
    out = k.dram("out", [NTC, DM], F32, kind="ExternalOutput")
    x0T_in = [k.dram(f"x0T_in{h}", [2048, NTC], F32) for h in range(2)]
    x0T_all = [k.dram(f"x0T_all{h}", [8 * 2048, NTC], F32) for h in range(2)]
    oT_part = [k.dram(f"oT_part{l}", [1024, SEQ], BF16) for l in range(2)]
    oT_all = [k.dram(f"oT_all{l}", [8 * 1024, SEQ], BF16) for l in range(2)]
    x1 = k.dram("x1_res", [NTC, DM], F32)
    x1T_loc = k.dram("x1T_loc", [DM, NTC], BF16); x1T_all = k.dram("x1T_all", [8 * DM, NTC], BF16)
    wo_in = [k.dram(f"wo_in{l}", [4 * 128, 4096], F32) for l in range(2)]; wo_all = [k.dram(f"wo_all{l}", [32 * 128, 4096], F32) for l in range(2)]
    LH = [(l, h) for l in range(2) for h in range(2)]
    wg_in = {lh: k.dram(f"wg_in{lh[0]}{lh[1]}", [1536, 4096], F32) for lh in LH}; wg_all = {lh: k.dram(f"wg_all{lh[0]}{lh[1]}", [8 * 1536, 4096], F32) for lh in LH}
    wu_in = {lh: k.dram(f"wu_in{lh[0]}{lh[1]}", [1536, 4096], F32) for lh in LH}; wu_all = {lh: k.dram(f"wu_all{lh[0]}{lh[1]}", [8 * 1536, 4096], F32) for lh in LH}
    wd_in = {lh: k.dram(f"wd_in{lh[0]}{lh[1]}", [2048, 3072], F32) for lh in LH}; wd_all = {lh: k.dram(f"wd_all{lh[0]}{lh[1]}", [8 * 2048, 3072], F32) for lh in LH}
    ident = k.sb("ident", [128, 128]); alphaI = k.sb("alphaI", [128, 128]); selm = k.sb("selm_sb", [128, 8]); selb = k.sb("selb_sb", [128, 2])
    xstg = k.sb("xstg", [128, 4, 512], BF16)
    k.dma('sp', ident[:], ident_d[:]); k.dma('sp', selm[:], selm_d[:]); k.dma('sp', selb[:], selb_d[:])
    k.ts(alphaI[:], ident[:], ALPHA, None, ALU.mult)
    consts = {'ident': ident, 'alphaI': alphaI}
    for h in range(2):
        k.dma('sp', x0T_in[h][:], xT_loc[h * 2048:(h + 1) * 2048, :])
        _collective(k, G8, x0T_in[h], x0T_all[h], f"x0T_all{h}", block=True)
    pending = []
    def gather_weights(l):
        def g0(l=l):
            k.dma('sp', wo_in[l][:], wo_loc[l])
            _collective(k, G8, wo_in[l], wo_all[l], f"wo_all{l}")
        pending.append(g0)
        for h in range(2):
            for (src, mid, dst, nm, rows) in ((wg_loc, wg_in, wg_all, "wg", 1536), (wu_loc, wu_in, wu_all, "wu", 1536), (wd_loc, wd_in, wd_all, "wd", 2048)):
                def g1(l=l, h=h, src=src, mid=mid, dst=dst, nm=nm, rows=rows):
                    k.dma('sp', mid[(l, h)][:], src[l][h * rows:(h + 1) * rows, :])
                    _collective(k, G8, mid[(l, h)], dst[(l, h)], f"{nm}_all{l}{h}")
                pending.append(g1)
    gather_weights(0); gather_weights(1)
    def tile_cb(ti):
        if pending: pending.pop(0)()
    if kstop == 0:
        while pending: pending.pop(0)()
    def finish():
        k.wait_all('sp', [out]); barrier(k); k.close(); return nc
    if kstop == 0: return finish()
    base = len(k.stack)
    def release():
        barrier(k)
        while len(k.stack) > base:
            k.stack.pop().__exit__(None, None, None)
    xsel = k.dram("xsel", [DM, SEQ], BF16)
    def xselect_pass(cands):
        mark = len(k.stack)
        stg = [[k.sb(f"xs{a}{bq}", [128, 16, 512], BF16) for bq in range(2)] for a in range(2)]
        it = 0
        for ti in range(SEQ // 512):
            t0 = ti * 512; r0 = t0 // NTC; c = t0 % NTC
            for hf in range(2):
                A, B = stg[it % 2]; it += 1
                k.dma('pool', A[:], cands(0, r0, c, 512, hf * 16, 16)); k.dma('pool', B[:], cands(1, r0, c, 512, hf * 16, 16))
                k.ts(A[:], A[:], selb[:, 0:1], None, ALU.mult)
                k.stt(A[:], B[:], selb[:, 1:2], A[:], ALU.mult, ALU.add)
                k.dma('sp', V(xsel, xsel.ap.rearrange("(k p) t -> p k t", p=128)[:, hf * 16:(hf + 1) * 16, t0:t0 + 512]), A[:])
        barrier(k)
        while len(k.stack) > mark: k.stack.pop().__exit__(None, None, None)
    def cands0(bb, r0, c, n, k0, nk):
        src_t = x0T_all[k0 // 16]
        return V(src_t, src_t.ap.rearrange("(r k p) t -> r p k t", r=8, p=128)[4 * bb + r0][:, :, c:c + n])
    def cands1(bb, r0, c, n, k0, nk):
        return V(x1T_all, x1T_all.ap.rearrange("(r k p) t -> r p k t", r=8, p=128)[4 * bb + r0][:, k0:k0 + nk, c:c + n])
    def xload_sel(dst, t0, n):
        k.dma('sp', dst[:], V(xsel, xsel.ap.rearrange("(k p) t -> p k t", p=128)[:, :, t0:t0 + n]))
    def xload0(dst, t0, n):
        r0 = t0 // NTC; c = t0 % NTC
        for kg in range(8):
            src_t = x0T_all[kg // 4]
            for bb in range(2):
                v = V(src_t, src_t.ap.rearrange("(r k p) t -> r p k t", r=8, p=128)[4 * bb + r0][:, (kg % 4) * 4:(kg % 4) * 4 + 4, c:c + n])
                k.dma('pool', xstg[:], v)
                if bb == 0: k.ts(dst[:, kg * 4:(kg + 1) * 4, :], xstg[:], selb[:, 0:1], None, ALU.mult)
                else: k.stt(dst[:, kg * 4:(kg + 1) * 4, :], xstg[:], selb[:, 1:2], dst[:, kg * 4:(kg + 1) * 4, :], ALU.mult, ALU.add)
    def xload1(dst, t0, n):
        r0 = t0 // NTC; c = t0 % NTC
        for kg in range(8):
            for bb in range(2):
                v = V(x1T_all, x1T_all.ap.rearrange("(r k p) t -> r p k t", r=8, p=128)[4 * bb + r0][:, kg * 4:(kg + 1) * 4, c:c + n])
                k.dma('pool', xstg[:], v)
                if bb == 0: k.ts(dst[:, kg * 4:(kg + 1) * 4, :], xstg[:], selb[:, 0:1], None, ALU.mult)
                else: k.stt(dst[:, kg * 4:(kg + 1) * 4, :], xstg[:], selb[:, 1:2], dst[:, kg * 4:(kg + 1) * 4, :], ALU.mult, ALU.add)
    def osrc(oall):
        def f(j, kg, t0, n):
            bb, ii = j // 4, j % 4
            return V(oall, oall.ap.rearrange("(r k p) t -> r p k t", r=8, p=128)[4 * bb + kg][:, :, ii * NTC + t0: ii * NTC + t0 + n])
        return f
    def wviews(l):
        wo = V(wo_all[l], wo_all[l].ap.rearrange("(b p) (k c) -> b p k c", p=128, c=128))
        class EW:
            def __init__(self, d, pat, kw): self.d = d; self.pat = pat; self.kw = kw
            def __getitem__(self, ej):
                e, j = ej
                t = self.d[(l, (e % 4) // 2)]
                return V(t, t.ap.rearrange(self.pat, **self.kw)[(e // 4) * 2 + (e % 2), j])
        wg = EW(wg_all, "(e j p) (k c) -> e j p k c", dict(j=6, p=128, c=128))
        wu = EW(wu_all, "(e j p) (k c) -> e j p k c", dict(j=6, p=128, c=128))
        wd = EW(wd_all, "(e b p) (j c) -> e b p j c", dict(b=8, p=128, c=512))
        return wo, wg, wu, wd
    xselect_pass(cands0)
    deltanet(k, SEQ, 512, xload_sel, dn_w, dn_wba, dn_cw, dn_al, dn_dtb, dn_nw,
             lambda t0, n: V(oT_part[0], oT_part[0].ap.rearrange("(h d) t -> d h t", d=128)[:, :, t0:t0 + n]), consts, tile_cb=tile_cb)
    while pending: pending.pop(0)()
    _collective(k, G8, oT_part[0], oT_all[0], "oT_all0")
    release()
    if kstop == 1: return finish()
    wo, wg, wu, wd = wviews(0)
    post_block(k, NTC, 512, osrc(oT_all[0]), xres, x1,
               lambda t0, n: V(x1T_loc, x1T_loc.ap.rearrange("(k p) t -> p k t", p=128)[:, :, t0:t0 + n]),
               wo, lng[0], lnb[0], rw[0], rb[0], wg, wu, wd, bg[0], bu[0], bd[0], consts, selmask=selm)
    _collective(k, G8, x1T_loc, x1T_all, "x1T_all")
    release()
    if kstop == 2: return finish()
    xselect_pass(cands1)
    moba(k, SEQ, 8, xload_sel, mb_w, lambda h, t0, n: oT_part[1][h * 128:(h + 1) * 128, t0:t0 + n], cd, consts)
    _collective(k, G8, oT_part[1], oT_all[1], "oT_all1")
    release()
    if kstop == 3: return finish()
    wo, wg, wu, wd = wviews(1)
    post_block(k, NTC, 512, osrc(oT_all[1]), x1, out, None,
               wo, lng[1], lnb[1], rw[1], rb[1], wg, wu, wd, bg[1], bu[1], bd[1], consts, selmask=selm)
    return finish()

def _lay_w(w):
    E = w.shape[0]
    return np.ascontiguousarray(w.reshape(E, 32, 128, 6, 128).transpose(0, 3, 2, 1, 4))
def _lay_wd(w):
    E = w.shape[0]
    return np.ascontiguousarray(w.reshape(E, 6, 128, 8, 512).transpose(0, 3, 2, 1, 4))

def kernel(x, dn_w_in, dn_conv_w, dn_a_log, dn_dt_bias, dn_norm_w, dn_w_out, mb_w_in, mb_w_out,
           ln_g, ln_b, router_w, router_b, w_gate, b_gate, w_up, b_up, w_down, b_down):
    f32 = lambda a: np.asarray(a, dtype=np.float32)
    x = f32(x); C = np.ascontiguousarray
    w_outs = [f32(dn_w_out)[0], f32(mb_w_out)[0]]
    wo_full = [C(w.reshape(32, 128, 32, 128).transpose(2, 1, 0, 3)) for w in w_outs]
    wgl = [_lay_w(f32(w_gate)[l]) for l in range(2)]; wul = [_lay_w(f32(w_up)[l]) for l in range(2)]; wdl = [_lay_wd(f32(w_down)[l]) for l in range(2)]
    shared = {
        "ident_d": np.eye(128, dtype=np.float32),
        "lng": C(f32(ln_g).reshape(2, 2, 32, 128).transpose(0, 1, 3, 2)), "lnb": C(f32(ln_b).reshape(2, 2, 32, 128).transpose(0, 1, 3, 2)),
        "rw": C(f32(router_w).reshape(2, 32, 128, 32).transpose(0, 2, 1, 3)),
        "rb": C(np.broadcast_to(f32(router_b)[:, None, :], (2, 128, 32))),
        "bg": C(f32(b_gate).reshape(2, 32, 6, 128).transpose(0, 3, 1, 2)), "bu": C(f32(b_up).reshape(2, 32, 6, 128).transpose(0, 3, 1, 2)),
        "bd": C(f32(b_down)),
    }
    dnw = f32(dn_w_in)[0]; cw = f32(dn_conv_w)[0]; mbw = f32(mb_w_in)[0]
    in_maps = []
    for r in range(8):
        b, i = r // 4, r % 4
        m = dict(shared)
        xs = x[b, i * NTC:(i + 1) * NTC]
        m["xres"] = C(xs); m["xT_loc"] = C(xs.T)
        sm = np.zeros((128, 8), np.float32); sm[:, r] = 1.0; m["selm"] = sm
        sb_ = np.zeros((128, 2), np.float32); sb_[:, b] = 1.0; m["selb"] = sb_
        hk = np.arange(4 * i, 4 * i + 4); hv = np.arange(8 * i, 8 * i + 8)
        cols = np.concatenate([(hk[:, None] * 128 + np.arange(128)).ravel(), 2048 + (hk[:, None] * 128 + np.arange(128)).ravel(),
                               4096 + (hv[:, None] * 128 + np.arange(128)).ravel(), 8192 + (hv[:, None] * 128 + np.arange(128)).ravel()])
        wc = dnw[:, cols]
        m["dn_w"] = C(wc.reshape(32, 128, 24, 128).transpose(2, 1, 0, 3))
        ba = dnw[:, np.concatenate([12288 + hv, 12320 + hv])]
        m["dn_wba"] = C(ba.reshape(32, 128, 16).transpose(1, 0, 2))
        m["dn_cw"] = C(cw[:, cols[:2048]].reshape(4, 16, 128).transpose(2, 1, 0))
        m["dn_al"] = C(np.broadcast_to(f32(dn_a_log)[0][hv], (64, 8))); m["dn_dtb"] = C(np.broadcast_to(f32(dn_dt_bias)[0][hv], (64, 8)))
        m["dn_nw"] = C(np.broadcast_to(f32(dn_norm_w)[0], (64, 128)))
        mcols = np.stack([np.stack([c0 + h * 128 + np.arange(128) for c0 in (0, 4096, 8192)], 0) for h in hv], 0)
        mw = mbw[:, mcols.reshape(-1)].reshape(32, 128, 8, 3, 128)
        m["mb_w"] = C(mw.transpose(2, 3, 1, 0, 4))
        m.update(moba_consts_host([int(h) for h in hv]))
        m["wo_loc"] = C(np.stack([wo_full[l][4 * r:4 * r + 4].reshape(4 * 128, 4096) for l in range(2)], 0))
        m["wg_loc"] = C(np.stack([wgl[l][4 * r:4 * r + 4].reshape(4 * 6 * 128, 4096) for l in range(2)], 0))
        m["wu_loc"] = C(np.stack([wul[l][4 * r:4 * r + 4].reshape(4 * 6 * 128, 4096) for l in range(2)], 0))
        m["wd_loc"] = C(np.stack([wdl[l][4 * r:4 * r + 4].reshape(4 * 8 * 128, 3072) for l in range(2)], 0))
        in_maps.append(m)
    nc = build_program(_KSTOP)
    res = run_bass_kernel_spmd(nc, in_maps, core_ids=list(range(8)))
    outp = np.empty((NB, SEQ, DM), np.float32)
    for r in range(8):
        b, i = r // 4, r % 4
        outp[b, i * NTC:(i + 1) * NTC] = res.results[r]["out"]
    return outp
```
